# Optimizing a Trainium2 kernel written in Bass

```python
import jax
import jax.numpy as jnp
from jax import lax
import numpy as np

D_MODEL = 4096
BATCH = 4
SEQ = 2048
DEPTH = 1

GRID_W = 64
CTX_LEN = 256
EPS = 1e-6
N_HEADS = 16
Q_RANK = 1024
KV_RANK = 512
NOPE_DIM = 128
ROPE_DIM = 64
V_DIM = 128
ROPE_BASE = 10000.0
Q_BLOCK = 128
CONV_DIM = 2048
CONV_WIDTH = 31
N_EXPERTS = 32
TOP_K = 4
EXPERT_FF = 1536
SWIGLU_LIMIT = 7.0
SWIGLU_ALPHA = 1.702

_KV0 = Q_RANK
_PE0 = _KV0 + KV_RANK
_CV0 = _PE0 + ROPE_DIM
_G0 = _CV0 + 2 * CONV_DIM
IN_COLS = _G0 + 2 * D_MODEL

kernel_name = "hybrid_mla_conformer_moe_block"


def rmsnorm(x, g):
    xf = x.astype(jnp.float32)
    y = xf * lax.rsqrt(jnp.mean(xf * xf, axis=-1, keepdims=True) + EPS)
    return (y * g.astype(jnp.float32)).astype(x.dtype)


def layernorm(x, g, b):
    xf = x.astype(jnp.float32)
    mu = jnp.mean(xf, axis=-1, keepdims=True)
    var = jnp.mean(jnp.square(xf - mu), axis=-1, keepdims=True)
    y = (xf - mu) * lax.rsqrt(var + EPS)
    return (y * g.astype(jnp.float32) + b.astype(jnp.float32)).astype(x.dtype)


def modulate(h, shift, scale):
    return h * (1 + scale) + shift


def axial_rope_tables(rows, dtype):
    row = jnp.broadcast_to(jnp.arange(rows, dtype=jnp.float32)[:, None], (rows, GRID_W)).reshape(-1)
    col = jnp.broadcast_to(jnp.arange(GRID_W, dtype=jnp.float32)[None, :], (rows, GRID_W)).reshape(-1)
    n_pairs = ROPE_DIM // 4
    inv_freq = ROPE_BASE ** (-jnp.arange(n_pairs, dtype=jnp.float32) / n_pairs)
    ang = jnp.concatenate([row[:, None] * inv_freq, col[:, None] * inv_freq], axis=-1)
    return jnp.cos(ang).astype(dtype), jnp.sin(ang).astype(dtype)


def apply_rope(x, cos, sin):
    half = ROPE_DIM // 2
    x1, x2 = x[..., :half], x[..., half:]
    return jnp.concatenate([x1 * cos - x2 * sin, x1 * sin + x2 * cos], axis=-1)


def mla_queries(q_lat, p):
    q = rmsnorm(q_lat, p['q_norm_g']) @ p['w_uq']
    q = q.reshape(q.shape[:-1] + (N_HEADS, NOPE_DIM + ROPE_DIM))
    return q[..., :NOPE_DIM], q[..., NOPE_DIM:]


def mla_keys_values(kv_lat, p):
    kv = rmsnorm(kv_lat, p['kv_norm_g']) @ p['w_ukv']
    kv = kv.reshape(kv.shape[:-1] + (N_HEADS, NOPE_DIM + V_DIM))
    return kv[..., :NOPE_DIM], kv[..., NOPE_DIM:]


def context_keys(hc, p):
    zk = hc @ p['w_in'][:, _KV0:_CV0]
    k_nope, v = mla_keys_values(zk[..., :KV_RANK], p)
    return k_nope, zk[..., KV_RANK:], v


def mla_attend(q_nope, q_pe, k_nope, k_pe, v):
    b, sq = q_nope.shape[:2]
    nblk = sq // Q_BLOCK
    scale = (NOPE_DIM + ROPE_DIM) ** -0.5

    def block(qs):
        qn, qp = qs
        s = jnp.einsum('bqhd,bkhd->bhqk', qn, k_nope) + jnp.einsum('bqhr,bkr->bhqk', qp, k_pe)
        prob = jax.nn.softmax(s.astype(jnp.float32) * scale, axis=-1).astype(v.dtype)
        return jnp.einsum('bhqk,bkhd->bqhd', prob, v)

    def to_blocks(t):
        return t.reshape((b, nblk, Q_BLOCK) + t.shape[2:]).swapaxes(0, 1)

    o = lax.map(block, (to_blocks(q_nope), to_blocks(q_pe)))
    return o.swapaxes(0, 1).reshape(b, sq, N_HEADS * V_DIM)


def conformer_conv(u, p):
    z = u[..., :CONV_DIM] * jax.nn.sigmoid(u[..., CONV_DIM:])
    z = lax.conv_general_dilated(
        z, p['w_dw'][:, None, :], window_strides=(1,),
        padding=[(CONV_WIDTH // 2, CONV_WIDTH // 2)],
        dimension_numbers=('NWC', 'WIO', 'NWC'),
        feature_group_count=CONV_DIM) + p['b_dw']
    z = jax.nn.silu(layernorm(z, p['cln_g'], p['cln_b']))
    return z @ p['w_pw'] + p['b_pw']


def token_mixer(h, rope, ctx_kv, p):
    z = h @ p['w_in']
    q_nope, q_pe = mla_queries(z[..., :_KV0], p)
    k_nope, v = mla_keys_values(z[..., _KV0:_PE0], p)
    k_pe = z[..., _PE0:_CV0]
    if rope is not None:
        cos, sin = rope
        q_pe = apply_rope(q_pe, cos[:, None, :], sin[:, None, :])
        k_pe = apply_rope(k_pe, cos, sin)
    if ctx_kv is not None:
        ck_nope, ck_pe, cv = ctx_kv
        k_nope = jnp.concatenate([ck_nope, k_nope], axis=1)
        k_pe = jnp.concatenate([ck_pe, k_pe], axis=1)
        v = jnp.concatenate([cv, v], axis=1)
    y_attn = mla_attend(q_nope, q_pe, k_nope, k_pe, v) @ p['w_o_attn']
    y_conv = conformer_conv(z[..., _CV0:_G0], p)
    gates = jax.nn.sigmoid(z[..., _G0:])
    merged = gates[..., :D_MODEL] * y_attn + gates[..., D_MODEL:] * y_conv
    return merged @ p['w_out']


def moe(h, p):
    b, s, d = h.shape
    t = h.reshape(b * s, d)
    logits = (t @ p['w_router'] + p['b_router']).astype(jnp.float32)
    top_val, top_idx = lax.top_k(logits, TOP_K)
    top_w = jax.nn.softmax(top_val, axis=-1)
    combine = jnp.sum(jax.nn.one_hot(top_idx, N_EXPERTS, dtype=jnp.float32) * top_w[..., None], axis=1)
    combine = combine.astype(h.dtype)

    def expert(acc, xs):
        wgu, bgu, wd, bd, ce = xs
        gu = t @ wgu + bgu
        g = jnp.minimum(gu[:, :EXPERT_FF], SWIGLU_LIMIT)
        u = jnp.clip(gu[:, EXPERT_FF:], -SWIGLU_LIMIT, SWIGLU_LIMIT)
        y = ((u + 1) * g * jax.nn.sigmoid(SWIGLU_ALPHA * g)) @ wd + bd
        return acc + ce[:, None] * y, None

    acc, _ = lax.scan(expert, jnp.zeros_like(t),
                      (p['w_gu'], p['b_gu'], p['w_down'], p['b_down'], combine.T))
    return acc.reshape(b, s, d)


def setup_inputs(seed: int = 0) -> dict:
    key = jax.random.key(seed)
    ks = iter(jax.random.split(key, 40))
    L = DEPTH

    def nrm(shape, scale):
        return jax.random.normal(next(ks), shape, jnp.float32) * scale

    def gain(shape):
        return 1.0 + nrm(shape, 0.05)

    return {
        'x': nrm((BATCH, SEQ, D_MODEL), 1.0),
        'c': nrm((BATCH, D_MODEL), 1.0),
        'ctx': nrm((BATCH, CTX_LEN, D_MODEL), 1.0),
        'c_ctx': nrm((D_MODEL,), 1.0),
        'w_ada': nrm((L, D_MODEL, 6 * D_MODEL), 0.5 * D_MODEL ** -0.5),
        'b_ada': nrm((L, 6 * D_MODEL), 0.02),
        'pre1_g': gain((L, D_MODEL)),
        'post1_g': gain((L, D_MODEL)),
        'pre2_g': gain((L, D_MODEL)),
        'post2_g': gain((L, D_MODEL)),
        'w_in': nrm((L, D_MODEL, IN_COLS), D_MODEL ** -0.5),
        'q_norm_g': gain((L, Q_RANK)),
        'w_uq': nrm((L, Q_RANK, N_HEADS * (NOPE_DIM + ROPE_DIM)), Q_RANK ** -0.5),
        'kv_norm_g': gain((L, KV_RANK)),
        'w_ukv': nrm((L, KV_RANK, N_HEADS * (NOPE_DIM + V_DIM)), KV_RANK ** -0.5),
        'w_o_attn': nrm((L, N_HEADS * V_DIM, D_MODEL), (N_HEADS * V_DIM) ** -0.5),
        'w_dw': nrm((L, CONV_WIDTH, CONV_DIM), CONV_WIDTH ** -0.5),
        'b_dw': nrm((L, CONV_DIM), 0.02),
        'cln_g': gain((L, CONV_DIM)),
        'cln_b': nrm((L, CONV_DIM), 0.02),
        'w_pw': nrm((L, CONV_DIM, D_MODEL), CONV_DIM ** -0.5),
        'b_pw': nrm((L, D_MODEL), 0.02),
        'w_out': nrm((L, D_MODEL, D_MODEL), D_MODEL ** -0.5),
        'w_router': nrm((L, D_MODEL, N_EXPERTS), D_MODEL ** -0.5),
        'b_router': nrm((L, N_EXPERTS), 0.01),
        'w_gu': nrm((L, N_EXPERTS, D_MODEL, 2 * EXPERT_FF), D_MODEL ** -0.5),
        'b_gu': nrm((L, N_EXPERTS, 2 * EXPERT_FF), 0.02),
        'w_down': nrm((L, N_EXPERTS, EXPERT_FF, D_MODEL), EXPERT_FF ** -0.5),
        'b_down': nrm((L, N_EXPERTS, D_MODEL), 0.02),
    }


def reference(x, c, ctx, c_ctx, w_ada, b_ada, pre1_g, post1_g, pre2_g, post2_g,
              w_in, q_norm_g, w_uq, kv_norm_g, w_ukv, w_o_attn,
              w_dw, b_dw, cln_g, cln_b, w_pw, b_pw, w_out,
              w_router, b_router, w_gu, b_gu, w_down, b_down):
    rows = x.shape[1] // GRID_W
    rope = axial_rope_tables(rows, x.dtype)
    for l in range(DEPTH):
        p = {
            'w_in': w_in[l], 'q_norm_g': q_norm_g[l], 'w_uq': w_uq[l],
            'kv_norm_g': kv_norm_g[l], 'w_ukv': w_ukv[l], 'w_o_attn': w_o_attn[l],
            'w_dw': w_dw[l], 'b_dw': b_dw[l], 'cln_g': cln_g[l], 'cln_b': cln_b[l],
            'w_pw': w_pw[l], 'b_pw': b_pw[l], 'w_out': w_out[l],
            'w_router': w_router[l], 'b_router': b_router[l],
            'w_gu': w_gu[l], 'b_gu': b_gu[l], 'w_down': w_down[l], 'b_down': b_down[l],
        }
        last = l == DEPTH - 1
        mod = jax.nn.silu(c) @ w_ada[l] + b_ada[l]
        sh1, sc1, g1, sh2, sc2, g2 = jnp.split(mod[:, None, :], 6, axis=-1)
        csh1, csc1, cg1, csh2, csc2, cg2 = jnp.split(jax.nn.silu(c_ctx) @ w_ada[l] + b_ada[l], 6)
        h = modulate(rmsnorm(x, pre1_g[l]), sh1, sc1)
        hc = modulate(rmsnorm(ctx, pre1_g[l]), csh1, csc1)
        y = token_mixer(h, rope, context_keys(hc, p), p)
        x = x + g1 * rmsnorm(y, post1_g[l])
        h2 = modulate(rmsnorm(x, pre2_g[l]), sh2, sc2)
        x = x + g2 * rmsnorm(moe(h2, p), post2_g[l])
        if not last:
            ctx = ctx + cg1 * rmsnorm(token_mixer(hc, None, None, p), post1_g[l])
            hc2 = modulate(rmsnorm(ctx, pre2_g[l]), csh2, csc2)
            ctx = ctx + cg2 * rmsnorm(moe(hc2, p), post2_g[l])
    return x
```

```python
import contextlib
import os
import numpy as np
import concourse.bass as bass
import concourse.mybir as mybir
from concourse.bass_utils import run_bass_kernel_spmd

F32 = mybir.dt.float32
BF16 = mybir.dt.bfloat16
AF = mybir.ActivationFunctionType
ALU = mybir.AluOpType

D = 4096
TOWN = 1024
TKV = 2304
TALL = 2336
NH = 16
NE = 32
FF = 1536
EPS = 1e-6
KV0, PE0, CV0, G0 = 1024, 1536, 1600, 5696
ENGS = ('tensor', 'vector', 'scalar', 'gpsimd', 'sync')
KSTOP = int(os.environ.get('KSTOP', '0'))
KNE = int(os.environ.get('KNE', '32'))
SEM_LIMIT = 1900


class Sm:
    def __init__(self, h, eng=None):
        self.h = h
        self.v = 0
        self.eng = eng


class Buf:
    def __init__(self, name='', excl=False):
        self.name = name
        self.excl = excl
        self.w = {}
        self.r = {}
        self.dsem = None


def _merge(dst, tok):
    sm, v = tok
    k = id(sm)
    if k not in dst or dst[k][1] < v:
        dst[k] = (sm, v)


class Ph:
    def __init__(self, nc, stack):
        self.nc = nc
        self.stack = stack
        self.q = {e: [] for e in ENGS}
        self.sems = []
        self.bufs = {}
        self.prog = {e: self.newsem('pg_' + e, e) for e in ENGS}
        self.waited = {e: {} for e in ENGS}
        self.dma_toks = {e: [] for e in ENGS}

    _n = [0]
    swpool = []
    outer = [None]

    def newswsem(self):
        return Ph.swpool.pop()

    def newsem(self, name, eng=None):
        Ph._n[0] += 1
        sm = Sm(self.stack.enter_context(self.nc.semaphore('%s_%d' % (name, Ph._n[0]))), eng)
        self.sems.append(sm)
        return sm

    def _wait(self, eng, tok):
        sm, v = tok
        if eng == 'tensor' and sm.eng == 'tensor':
            return
        k = id(sm)
        if self.waited[eng].get(k, 0) >= v:
            return
        self.waited[eng][k] = v
        self.q[eng].append(('wait', sm, v))

    def _deps(self, eng, reads, writes):
        for b in list(reads) + list(writes):
            self.bufs[id(b)] = b
        for b in reads:
            for t in b.w.values():
                self._wait(eng, t)
            if b.excl:
                for t in b.r.values():
                    self._wait(eng, t)
        for b in writes:
            for t in b.w.values():
                self._wait(eng, t)
            for t in b.r.values():
                self._wait(eng, t)

    def op(self, eng, fn, reads=(), writes=(), sig=True):
        self._deps(eng, reads, writes)
        tok = None
        if sig:
            if self.prog[eng].v >= SEM_LIMIT:
                self.prog[eng] = self.newsem('pg_' + eng, eng)
            sm = self.prog[eng]
            sm.v += 1
            tok = (sm, sm.v)
            for b in reads:
                _merge(b.r, tok)
            for b in writes:
                _merge(b.w, tok)
        self.q[eng].append(('op', fn, tok, 1))
        return tok

    def dma(self, eng, out, in_, reads=(), writes=(), **kw):
        self._deps(eng, reads, writes)
        tgt = writes[0] if writes else reads[0]
        if tgt.dsem is None:
            tgt.dsem = {}
        kind = 'sw' if eng == 'gpsimd' else 'hw'
        if kind not in tgt.dsem or tgt.dsem[kind].v >= SEM_LIMIT:
            tgt.dsem[kind] = self.newswsem() if kind == 'sw' else self.newsem('d' + kind)
        sm = tgt.dsem[kind]
        sm.v += 16
        tok = (sm, sm.v)
        for b in reads:
            _merge(b.r, tok)
        for b in writes:
            _merge(b.w, tok)
        self.q[eng].append(('op', lambda e, out=out, in_=in_, kw=kw: e.dma_start(out=out, in_=in_, **kw), tok, 16))
        self.dma_toks[eng].append(tok)
        return tok

    def mm(self, out, lhsT, rhs, start, stop, reads=(), writes=(), sig=False):
        return self.op('tensor', lambda e: e.matmul(out, lhsT, rhs, start=start, stop=stop),
                       reads, writes, sig)

    def tr(self, out, in_, ident, reads=(), writes=(), sig=False):
        return self.op('tensor', lambda e: e.transpose(out, in_, ident), reads, writes, sig)

    def act(self, out, in_, func, reads=(), writes=(), bias=None, scale=None, accum_out=None):
        kw = {}
        if bias is not None:
            kw['bias'] = bias
        if scale is not None:
            kw['scale'] = scale
        if accum_out is not None:
            kw['accum_out'] = accum_out
        return self.op('scalar', lambda e: e.activation(out=out, in_=in_, func=func, **kw), reads, writes)

    def tt(self, eng, out, in0, in1, op, reads=(), writes=()):
        return self.op(eng, lambda e: e.tensor_tensor(out=out, in0=in0, in1=in1, op=op), reads, writes)

    def ts(self, eng, out, in0, s1, op0, s2=None, op1=None, reads=(), writes=()):
        if op1 is None:
            return self.op(eng, lambda e: e.tensor_scalar(out=out, in0=in0, scalar1=s1, scalar2=None, op0=op0),
                           reads, writes)
        return self.op(eng, lambda e: e.tensor_scalar(out=out, in0=in0, scalar1=s1, scalar2=s2, op0=op0, op1=op1),
                       reads, writes)

    def stt(self, out, in0, scalar, in1, op0, op1, reads=(), writes=()):
        return self.op('vector', lambda e: e.scalar_tensor_tensor(out=out, in0=in0, scalar=scalar, in1=in1,
                                                                   op0=op0, op1=op1), reads, writes)

    def cp(self, eng, out, in_, reads=(), writes=()):
        if eng == 'scalar':
            return self.op(eng, lambda e: e.copy(out=out, in_=in_), reads, writes)
        return self.op(eng, lambda e: e.tensor_copy(out=out, in_=in_), reads, writes)

    def recip(self, out, in_, reads=(), writes=()):
        return self.op('vector', lambda e: e.reciprocal(out=out, in_=in_), reads, writes)

    def memset(self, eng, ap, val, writes=()):
        return self.op(eng, lambda e: e.memset(ap, val), (), writes)

    def emit(self):
        for eng in ENGS:
            last = {}
            for sm, v in self.dma_toks[eng]:
                last[id(sm)] = (sm, v)
            for sm, v in last.values():
                self.q[eng].append(('wait', sm, v))
        sems = self.sems
        with self.nc.Block() as block0:
            def clr(e, sems=sems):
                for sm in sems:
                    e.sem_clear(sm.h)
            block0.sync(clr)
        with self.nc.Block() as block:
            for eng in ENGS:
                items = self.q[eng]
                if not items:
                    continue

                def body(e, items=items):
                    for it in items:
                        if it[0] == 'wait':
                            e.wait_ge(it[1].h, it[2])
                        else:
                            ins = it[1](e)
                            if it[2] is not None:
                                ins.then_inc(it[2][0].h, it[3])
                getattr(block, eng)(body)
        for b in self.bufs.values():
            b.w = {}
            b.r = {}
            b.dsem = None


def build_nc(debug=(), upto=None, big=True):
    nc = bass.Bass("TRN2", target_bir_lowering=False)

    def din(name, shape, dt=F32):
        return nc.dram_tensor(name, list(shape), dt, kind="ExternalInput").ap()

    def dscr(name, shape, dt):
        kind = "ExternalOutput" if name in debug else "Internal"
        return nc.dram_tensor(name, list(shape), dt, kind=kind).ap()

    xt_d = din("xt", [TALL, D])
    cvT_d = din("cvT", [128, 32, 2])
    hmask_d = din("hmask", [1, 32])
    cos_d = din("cosT", [64, TKV])
    sin_d = din("sinT", [64, TKV])
    ident_d = din("ident", [128, 128])
    w_ada = din("w_ada", [D, 6 * D])
    b_ada = din("b_ada", [6, D])
    gains = din("gains", [4, D])
    w_in = din("w_in", [D, 13888])
    qng_d = din("qng", [128, 8])
    w_uq = din("w_uq", [1024, 3072])
    kvg_d = din("kvg", [128, 4])
    w_ukv = din("w_ukv", [512, 4096])
    w_oa = din("w_o_attn", [2048, D])
    wdwT_d = din("wdwT", [128, 16, 31])
    cvec3_d = din("cvec3", [128, 3, 16])
    w_pw = din("w_pw", [2048, D])
    bpw_d = din("bpw", [128, 32])
    w_out = din("w_out", [D, D])
    w_router = din("w_router", [D, NE])
    b_router = din("b_router", [1, NE])
    w_gu = din("w_gu", [KNE, D, 2 * FF] if big else [NE, 128, 128])
    bgu_d = din("bgu", [128, NE, 24])
    w_down = din("w_down", [KNE * FF, D] if big else [NE * FF, 128])
    b_down = din("b_down", [NE, D])
    out_d = nc.dram_tensor("out", [TOWN, D], F32, kind="ExternalOutput").ap()

    modv_d = dscr("modv", [6, 2, D], F32)
    hT_d = dscr("hT", [D, TALL], BF16)
    kT_d = dscr("kT", [NH, 128, TKV], BF16)
    kpeT_d = dscr("kpeT", [64, TKV], BF16)
    v_d = dscr("vv", [TKV, 2048], BF16)
    qn_d = dscr("qn", [1024, TOWN], BF16)
    qTn_d = dscr("qTn", [NH, 128, TOWN], BF16)
    qTr_d = dscr("qTr", [NH, 64, TOWN], BF16)
    attnT_d = dscr("attnT", [2048, TOWN], BF16)
    caT_d = dscr("caT", [2048, TOWN], BF16)
    mT_d = dscr("mT", [D, TOWN], BF16)
    x1_d = dscr("x1", [TOWN, D], F32)
    h2T_d = dscr("h2T", [D, TOWN], BF16)
    comb_d = dscr("comb", [NE, TOWN], F32)
    combb_d = dscr("combb", [NE, TOWN], BF16)
    actT_d = dscr("actT", [KNE * FF, TOWN], BF16)
    ymoe_d = dscr("ymoe", [TOWN, D], F32)

    B_modv, B_hT, B_kTd, B_kpeT, B_v, B_qn, B_qTn, B_qTr = (Buf() for _ in range(8))
    B_attnT, B_caT, B_mT, B_x1, B_h2T, B_comb, B_combb, B_actT, B_ymoe, B_out = (Buf() for _ in range(10))

    hT_v = hT_d.rearrange("(k p) t -> p k t", p=128)
    w_in_v = w_in.rearrange("(k p) n -> p k n", p=128)

    with contextlib.ExitStack() as stack:
        Ph.swpool = []
        Ph.outer[0] = stack

        def sb(name, shape, dt):
            return stack.enter_context(nc.sbuf_tensor(name, list(shape), dt))

        identf = sb("identf", [128, 128], F32)
        identb = sb("identb", [128, 128], BF16)
        onesb = sb("onesb", [128, 128], BF16)
        epst = sb("epst", [128, 1], F32)
        B_const = Buf()

        P = Ph(nc, stack)
        Ph.swpool = [Sm(stack.enter_context(nc.semaphore('dsw_%d' % i))) for i in range(44)]
        P.sems.extend(Ph.swpool)
        P.dma('sync', identf[:], ident_d, writes=[B_const])
        P.dma('gpsimd', identb[:], ident_d, writes=[B_const])
        P.memset('vector', onesb[:], 1.0, writes=[B_const])
        P.memset('vector', epst[:], EPS, writes=[B_const])
        P.emit()

        with contextlib.ExitStack() as st:
            def sbl(name, shape, dt, st=st):
                return st.enter_context(nc.sbuf_tensor(name, list(shape), dt))
            P = Ph(nc, st)
            cv = sbl("a_cv", [128, 32, 2], F32)
            sT = sbl("a_sT", [128, 32, 2], BF16)
            wt = sbl("a_wt", [128, 2, 32, 512], BF16)
            modc = sbl("a_modc", [2, D], F32)
            bac = sbl("a_bac", [2, D], F32)
            gnc = sbl("a_gnc", [2, D], F32)
            ps = st.enter_context(nc.psum_tensor("a_ps", [128, 8, 512], F32))
            B_cv, B_sT, B_modc, B_bac, B_gnc = Buf(), Buf(), Buf(), Buf(), Buf()
            B_wt = [Buf(), Buf()]
            B_ps = [Buf(excl=True) for _ in range(8)]
            P.dma('sync', cv[:], cvT_d, writes=[B_cv])
            P.act(sT[:], cv[:], AF.Silu, reads=[B_cv], writes=[B_sT])
            w_ada_v = w_ada.rearrange("(k p) n -> p k n", p=128)
            gain_of = {1: 0, 2: 1, 4: 2, 5: 3}
            for ci in range(6):
                P.dma('sync', bac[:], b_ada[ci:ci + 1, :].broadcast_to([2, D]), writes=[B_bac])
                if ci in gain_of:
                    g = gain_of[ci]
                    P.dma('sync', gnc[:], gains[g:g + 1, :].broadcast_to([2, D]), writes=[B_gnc])
                for t8 in range(8):
                    t = ci * 8 + t8
                    P.dma('gpsimd', wt[:, t % 2], w_ada_v[:, :, t * 512:(t + 1) * 512], writes=[B_wt[t % 2]])
                    for k in range(32):
                        P.mm(ps[0:2, t % 8, :], sT[:, k, :], wt[:, t % 2, k, :], k == 0, k == 31,
                             reads=[B_sT, B_wt[t % 2]], writes=[B_ps[t % 8]], sig=(k == 31))
                    P.tt('vector', modc[:, t8 * 512:(t8 + 1) * 512], ps[0:2, t % 8, :],
                         bac[:, t8 * 512:(t8 + 1) * 512], ALU.add,
                         reads=[B_ps[t % 8], B_bac], writes=[B_modc])
                if ci in (1, 4):
                    P.stt(modc[:], modc[:], 1.0, gnc[:], ALU.add, ALU.mult, reads=[B_gnc], writes=[B_modc])
                elif ci in (2, 5):
                    P.tt('vector', modc[:], modc[:], gnc[:], ALU.mult, reads=[B_gnc], writes=[B_modc])
                P.dma('sync', modv_d[ci], modc[:], reads=[B_modc], writes=[B_modv])
            P.emit()
            if upto == 'A':
                return nc

        with contextlib.ExitStack() as st:
            def sbl(name, shape, dt, st=st):
                return st.enter_context(nc.sbuf_tensor(name, list(shape), dt))
            P = Ph(nc, st)
            Ab = sbl("b_Ab", [128, D], F32)
            Bb = sbl("b_Bb", [128, D], F32)
            Ac = sbl("b_Ac", [128, D], F32)
            Bc = sbl("b_Bc", [128, D], F32)
            xt = sbl("b_xt", [128, 2, D], F32)
            hb = sbl("b_hb", [128, 2, D], BF16)
            junk = sbl("b_junk", [128, D], BF16)
            stat = sbl("b_stat", [128, 3, 32], F32)
            hTs = sbl("b_hTs", [128, 32, 512], BF16)
            pT = st.enter_context(nc.psum_tensor("b_pT", [128, 8, 8, 128], BF16))
            B_mod4, B_junk, B_stat, B_hTs = Buf(), Buf(), Buf(), Buf()
            B_xt = [Buf(), Buf()]
            B_hb = [Buf(), Buf()]
            B_pT = [Buf(excl=True) for _ in range(8)]
            P.dma('sync', Ab[:], modv_d[1, 0:1, :].broadcast_to([128, D]), reads=[B_modv], writes=[B_mod4])
            P.dma('sync', Bb[:], modv_d[0, 0:1, :].broadcast_to([128, D]), reads=[B_modv], writes=[B_mod4])
            P.dma('sync', Ac[:], modv_d[1, 1:2, :].broadcast_to([128, D]), reads=[B_modv], writes=[B_mod4])
            P.dma('sync', Bc[:], modv_d[0, 1:2, :].broadcast_to([128, D]), reads=[B_modv], writes=[B_mod4])
            groups = [(0, 4), (4, 4), (8, 4), (12, 4), (16, 2), (18, 1)]
            ti = 0
            bank = 0
            for g0, gn in groups:
                for tl in range(gn):
                    tile = g0 + tl
                    np_ = 128 if tile < 18 else 32
                    s = ti % 2
                    isctx = tile in (16, 17)
                    A_, B_ = (Ac, Bc) if isctx else (Ab, Bb)
                    P.dma('sync', xt[0:np_, s, :], xt_d[tile * 128:tile * 128 + np_, :], writes=[B_xt[s]])
                    np_ = 128
                    P.act(junk[0:np_, :], xt[0:np_, s, :], AF.Square, reads=[B_xt[s]], writes=[B_junk, B_stat],
                          accum_out=stat[0:np_, 0, ti:ti + 1])
                    P.act(stat[0:np_, 1, ti:ti + 1], stat[0:np_, 0, ti:ti + 1], AF.Sqrt, reads=[B_stat],
                          writes=[B_stat], bias=epst[0:np_, :], scale=1.0 / D)
                    P.recip(stat[0:np_, 2, ti:ti + 1], stat[0:np_, 1, ti:ti + 1], reads=[B_stat], writes=[B_stat])
                    P.stt(xt[0:np_, s, :], xt[0:np_, s, :], stat[0:np_, 2, ti:ti + 1], A_[0:np_, :], ALU.mult,
                          ALU.mult, reads=[B_stat, B_mod4], writes=[B_xt[s]])
                    P.tt('gpsimd', hb[0:np_, s, :], xt[0:np_, s, :], B_[0:np_, :], ALU.add,
                         reads=[B_xt[s], B_mod4], writes=[B_hb[s]])
                    for q4 in range(4):
                        bk = bank % 8
                        bank += 1
                        for j8 in range(8):
                            j = q4 * 8 + j8
                            P.tr(pT[:, bk, j8, 0:np_], hb[0:np_, s, j * 128:(j + 1) * 128], identb[0:np_, 0:np_],
                                 reads=[B_hb[s]], writes=[B_pT[bk]], sig=(j8 == 7))
                        P.cp('scalar' if q4 % 2 == 0 else 'vector',
                             hTs[:, q4 * 8:(q4 + 1) * 8, tl * 128:tl * 128 + np_], pT[:, bk, :, 0:np_],
                             reads=[B_pT[bk]], writes=[B_hTs])
                    ti += 1
                t0 = g0 * 128
                nt = gn * 128 if g0 < 18 else 32
                P.dma('sync', hT_v[:, :, t0:t0 + nt], hTs[:, :, 0:nt], reads=[B_hTs], writes=[B_hT])
            P.emit()
            if upto == 'B':
                return nc

        with contextlib.ExitStack() as st:
            def sbl(name, shape, dt, st=st):
                return st.enter_context(nc.sbuf_tensor(name, list(shape), dt))
            P = Ph(nc, st)
            wkv = sbl("c1_wkv", [128, 32, 512], BF16)
            wpr = sbl("c1_wpr", [128, 2, 32, 128], BF16)
            wuk = sbl("c1_wuk", [128, 4, 16, 128], BF16)
            wuv = sbl("c1_wuv", [128, 4, 16, 128], BF16)
            kvg = sbl("c1_kvg", [128, 4], F32)
            cs = sbl("c1_cs", [64, 2, 512], F32)
            hTb = sbl("c1_hTb", [128, 2, 32, 512], BF16)
            kvf = sbl("c1_kvf", [128, 4, 512], F32)
            sq = sbl("c1_sq", [128, 4, 512], BF16)
            rs = sbl("c1_rs", [128, 2, 512], F32)
            kvn = sbl("c1_kvn", [128, 4, 512], BF16)
            rp = sbl("c1_rp", [64, 2, 512], F32)
            kpo = sbl("c1_kpo", [64, 512], BF16)
            kTs = sbl("c1_kTs", [128, 16, 512], BF16)
            vs = sbl("c1_vs", [128, 4, 2048], BF16)
            ps = st.enter_context(nc.psum_tensor("c1_ps", [128, 8, 512], F32))
            B_w, B_cs, B_kvf, B_sq, B_rs, B_kvn, B_rp, B_kpo, B_kTs, B_vs = (Buf() for _ in range(10))
            B_hTb = [Buf(), Buf()]
            B_ps = [Buf(excl=True) for _ in range(8)]
            P.dma('gpsimd', wkv[:], w_in_v[:, :, KV0:PE0], writes=[B_w])
            P.memset('vector', wpr[:], 0.0, writes=[B_w])
            P.dma('gpsimd', wpr[:, 0, :, 0:64], w_in_v[:, :, PE0:PE0 + 64], writes=[B_w])
            P.dma('gpsimd', wpr[:, 1, :, 0:32], w_in_v[:, :, PE0 + 32:PE0 + 64], writes=[B_w])
            P.dma('gpsimd', wpr[:, 1, :, 32:64], w_in_v[:, :, PE0:PE0 + 32], writes=[B_w])
            P.op('scalar', lambda e: e.mul(out=wpr[:, 1, :, 0:32], in_=wpr[:, 1, :, 0:32], mul=-1.0), writes=[B_w])
            ukv_v = w_ukv.rearrange("(j p) (h two d) -> p j h two d", p=128, two=2, d=128)
            for j in range(4):
                P.dma('gpsimd', wuk[:, j], ukv_v[:, j, :, 0, :], writes=[B_w])
                P.dma('gpsimd', wuv[:, j], ukv_v[:, j, :, 1, :], writes=[B_w])
            P.dma('sync', kvg[:], kvg_d, writes=[B_w])
            pb = [0]

            def nb():
                pb[0] += 1
                return pb[0] % 8
            blocks = [(0, 512), (512, 512), (1024, 512), (1536, 512), (2048, 256)]
            for bi, (t0, nt) in enumerate(blocks):
                if KSTOP == 1 or (KSTOP and bi == 1):
                    break
                s = bi % 2
                P.dma('sync', hTb[:, s, :, 0:nt], hT_v[:, :, t0:t0 + nt], reads=[B_hT], writes=[B_hTb[s]])
                P.dma('sync', cs[:, 0, 0:nt], cos_d[:, t0:t0 + nt], writes=[B_cs])
                P.dma('sync', cs[:, 1, 0:nt], sin_d[:, t0:t0 + nt], writes=[B_cs])
                for j in range(4):
                    b = nb()
                    for k in range(32):
                        P.mm(ps[:, b, 0:nt], wkv[:, k, j * 128:(j + 1) * 128], hTb[:, s, k, 0:nt], k == 0, k == 31,
                             reads=[B_w, B_hTb[s]], writes=[B_ps[b]], sig=(k == 31))
                    P.cp('vector', kvf[:, j, 0:nt], ps[:, b, 0:nt], reads=[B_ps[b]], writes=[B_kvf])
                    P.act(sq[:, j, 0:nt], ps[:, b, 0:nt], AF.Square, reads=[B_ps[b]], writes=[B_sq])
                if KSTOP == 2:
                    break
                b = nb()
                for j in range(4):
                    P.mm(ps[:, b, 0:nt], onesb[:], sq[:, j, 0:nt], j == 0, j == 3, reads=[B_sq, B_const],
                         writes=[B_ps[b]], sig=(j == 3))
                P.act(rs[:, 0, 0:nt], ps[:, b, 0:nt], AF.Sqrt, reads=[B_ps[b]], writes=[B_rs], bias=epst[:],
                      scale=1.0 / 512)
                P.recip(rs[:, 1, 0:nt], rs[:, 0, 0:nt], reads=[B_rs], writes=[B_rs])
                for j in range(4):
                    P.stt(kvn[:, j, 0:nt], kvf[:, j, 0:nt], kvg[:, j:j + 1], rs[:, 1, 0:nt], ALU.mult, ALU.mult,
                          reads=[B_kvf, B_rs, B_w], writes=[B_kvn])
                if KSTOP == 3:
                    break
                b1 = nb()
                for k in range(32):
                    P.mm(ps[:, b1, 0:nt], wpr[:, 0, k, :], hTb[:, s, k, 0:nt], k == 0, k == 31,
                         reads=[B_w, B_hTb[s]], writes=[B_ps[b1]], sig=(k == 31))
                b2 = nb()
                for k in range(32):
                    P.mm(ps[:, b2, 0:nt], wpr[:, 1, k, :], hTb[:, s, k, 0:nt], k == 0, k == 31,
                         reads=[B_w, B_hTb[s]], writes=[B_ps[b2]], sig=(k == 31))
                P.tt('vector', rp[:, 0, 0:nt], ps[0:64, b1, 0:nt], cs[:, 0, 0:nt], ALU.mult,
                     reads=[B_ps[b1], B_cs], writes=[B_rp])
                P.tt('vector', rp[:, 1, 0:nt], ps[0:64, b2, 0:nt], cs[:, 1, 0:nt], ALU.mult,
                     reads=[B_ps[b2], B_cs], writes=[B_rp])
                P.tt('gpsimd', kpo[:, 0:nt], rp[:, 0, 0:nt], rp[:, 1, 0:nt], ALU.add, reads=[B_rp], writes=[B_kpo])
                P.dma('sync', kpeT_d[:, t0:t0 + nt], kpo[:, 0:nt], reads=[B_kpo], writes=[B_kpeT])
                if KSTOP == 4:
                    break
                for h in range(NH):
                    b = nb()
                    for j in range(4):
                        P.mm(ps[:, b, 0:nt], wuk[:, j, h, :], kvn[:, j, 0:nt], j == 0, j == 3,
                             reads=[B_w, B_kvn], writes=[B_ps[b]], sig=(j == 3))
                    P.cp('scalar' if h % 2 == 0 else 'vector', kTs[:, h, 0:nt], ps[:, b, 0:nt],
                         reads=[B_ps[b]], writes=[B_kTs])
                P.dma('sync', kT_d.rearrange("h p t -> p h t")[:, :, t0:t0 + nt], kTs[:, :, 0:nt],
                      reads=[B_kTs], writes=[B_kTd])
                if KSTOP == 5:
                    break
                nsub = nt // 128
                for su in range(nsub):
                    for g in range(4):
                        b = nb()
                        for j in range(4):
                            P.mm(ps[:, b, :], kvn[:, j, su * 128:(su + 1) * 128],
                                 wuv[:, j, g * 4:(g + 1) * 4, :], j == 0, j == 3,
                                 reads=[B_w, B_kvn], writes=[B_ps[b]], sig=(j == 3))
                        P.cp('scalar' if g % 2 == 0 else 'vector', vs[:, su, g * 512:(g + 1) * 512], ps[:, b, :],
                             reads=[B_ps[b]], writes=[B_vs])
                P.dma('sync', v_d[t0:t0 + nt, :].rearrange("(s p) c -> p s c", p=128), vs[:, 0:nsub, :],
                      reads=[B_vs], writes=[B_v])
            P.emit()
            if upto == 'C1':
                return nc

        with contextlib.ExitStack() as st:
            def sbl(name, shape, dt, st=st):
                return st.enter_context(nc.sbuf_tensor(name, list(shape), dt))
            P = Ph(nc, st)
            wq = sbl("c2_wq", [128, 32, 1024], BF16)
            qng = sbl("c2_qng", [128, 8], F32)
            hTb = sbl("c2_hTb", [128, 2, 32, 512], BF16)
            qf = sbl("c2_qf", [128, 8, 512], F32)
            sq = sbl("c2_sq", [128, 8, 512], BF16)
            rs = sbl("c2_rs", [128, 2, 512], F32)
            qno = sbl("c2_qno", [128, 8, 512], BF16)
            ps = st.enter_context(nc.psum_tensor("c2_ps", [128, 8, 512], F32))
            B_w, B_qf, B_sq, B_rs, B_qno = (Buf() for _ in range(5))
            B_hTb = [Buf(), Buf()]
            B_ps = [Buf(excl=True) for _ in range(8)]
            P.dma('gpsimd', wq[:, :, 0:512], w_in_v[:, :, 0:512], writes=[B_w])
            P.dma('gpsimd', wq[:, :, 512:1024], w_in_v[:, :, 512:1024], writes=[B_w])
            P.dma('sync', qng[:], qng_d, writes=[B_w])
            pbc = 0
            for bi in range(2):
                t0 = bi * 512
                P.dma('sync', hTb[:, bi], hT_v[:, :, t0:t0 + 512], reads=[B_hT], writes=[B_hTb[bi]])
                for j in range(8):
                    pbc += 1
                    b = pbc % 8
                    for k in range(32):
                        P.mm(ps[:, b, :], wq[:, k, j * 128:(j + 1) * 128], hTb[:, bi, k, :], k == 0, k == 31,
                             reads=[B_w, B_hTb[bi]], writes=[B_ps[b]], sig=(k == 31))
                    P.cp('vector', qf[:, j, :], ps[:, b, :], reads=[B_ps[b]], writes=[B_qf])
                    P.act(sq[:, j, :], ps[:, b, :], AF.Square, reads=[B_ps[b]], writes=[B_sq])
                pbc += 1
                b = pbc % 8
                for j in range(8):
                    P.mm(ps[:, b, :], onesb[:], sq[:, j, :], j == 0, j == 7, reads=[B_sq, B_const],
                         writes=[B_ps[b]], sig=(j == 7))
                P.act(rs[:, 0, :], ps[:, b, :], AF.Sqrt, reads=[B_ps[b]], writes=[B_rs], bias=epst[:],
                      scale=1.0 / 1024)
                P.recip(rs[:, 1, :], rs[:, 0, :], reads=[B_rs], writes=[B_rs])
                for j in range(8):
                    P.stt(qno[:, j, :], qf[:, j, :], qng[:, j:j + 1], rs[:, 1, :], ALU.mult, ALU.mult,
                          reads=[B_qf, B_rs, B_w], writes=[B_qno])
                P.dma('sync', qn_d.rearrange("(j p) t -> p j t", p=128)[:, :, t0:t0 + 512], qno[:],
                      reads=[B_qno], writes=[B_qn])
            P.emit()
            if upto == 'C2a':
                return nc

        SCALE = float((128 + 64) ** -0.5)
        with contextlib.ExitStack() as st:
            def sbl(name, shape, dt, st=st):
                return st.enter_context(nc.sbuf_tensor(name, list(shape), dt))
            P = Ph(nc, st)
            wn = sbl("c3_wn", [128, 8, 16, 128], BF16)
            wr = sbl("c3_wr", [128, 8, 16, 128], BF16)
            wrr = sbl("c3_wrr", [128, 8, 16, 128], BF16)
            qn = sbl("c3_qn", [128, 8, TOWN], BF16)
            cs = sbl("c3_cs", [64, 2, TOWN], F32)
            rp = sbl("c3_rp", [64, 2, 512], F32)
            qsn = sbl("c3_qsn", [128, 16, 512], BF16)
            qsr = sbl("c3_qsr", [64, 16, 512], BF16)
            ps = st.enter_context(nc.psum_tensor("c3_ps", [128, 8, 512], F32))
            B_w, B_qnb, B_cs, B_rp, B_qsn, B_qsr = (Buf() for _ in range(6))
            B_ps = [Buf(excl=True) for _ in range(8)]
            uq_v = w_uq.rearrange("(j p) (h c) -> p j h c", p=128, c=192)
            P.memset('vector', wr[:], 0.0, writes=[B_w])
            P.memset('gpsimd', wrr[:], 0.0, writes=[B_w])
            for j in range(8):
                P.dma('gpsimd', wn[:, j], uq_v[:, j, :, 0:128], writes=[B_w])
                P.dma('gpsimd', wr[:, j, :, 0:64], uq_v[:, j, :, 128:192], writes=[B_w])
                P.dma('gpsimd', wrr[:, j, :, 0:32], uq_v[:, j, :, 160:192], writes=[B_w])
                P.dma('gpsimd', wrr[:, j, :, 32:64], uq_v[:, j, :, 128:160], writes=[B_w])
            P.op('scalar', lambda e: e.mul(out=wrr[:, :, :, 0:32], in_=wrr[:, :, :, 0:32], mul=-1.0), writes=[B_w])
            P.dma('sync', qn[:], qn_d.rearrange("(j p) t -> p j t", p=128), reads=[B_qn], writes=[B_qnb])
            P.dma('sync', cs[:, 0, :], cos_d[:, 0:TOWN], writes=[B_cs])
            P.dma('sync', cs[:, 1, :], sin_d[:, 0:TOWN], writes=[B_cs])
            pbc = 0
            for bi in range(2):
                t0 = bi * 512
                for h in range(NH):
                    pbc += 1
                    b = pbc % 8
                    for j in range(8):
                        P.mm(ps[:, b, :], wn[:, j, h, :], qn[:, j, t0:t0 + 512], j == 0, j == 7,
                             reads=[B_w, B_qnb], writes=[B_ps[b]], sig=(j == 7))
                    P.act(qsn[:, h, :], ps[:, b, :], AF.Identity, reads=[B_ps[b]], writes=[B_qsn], scale=SCALE)
                    pbc += 1
                    b1 = pbc % 8
                    for j in range(8):
                        P.mm(ps[:, b1, :], wr[:, j, h, :], qn[:, j, t0:t0 + 512], j == 0, j == 7,
                             reads=[B_w, B_qnb], writes=[B_ps[b1]], sig=(j == 7))
                    pbc += 1
                    b2 = pbc % 8
                    for j in range(8):
                        P.mm(ps[:, b2, :], wrr[:, j, h, :], qn[:, j, t0:t0 + 512], j == 0, j == 7,
                             reads=[B_w, B_qnb], writes=[B_ps[b2]], sig=(j == 7))
                    P.tt('vector', rp[:, 0, :], ps[0:64, b1, :], cs[:, 0, t0:t0 + 512], ALU.mult,
                         reads=[B_ps[b1], B_cs], writes=[B_rp])
                    P.stt(rp[:, 1, :], ps[0:64, b2, :], SCALE, cs[:, 1, t0:t0 + 512], ALU.mult, ALU.mult,
                          reads=[B_ps[b2], B_cs], writes=[B_rp])
                    P.stt(qsr[:, h, :], rp[:, 0, :], SCALE, rp[:, 1, :], ALU.mult, ALU.add, reads=[B_rp],
                          writes=[B_qsr])
                P.dma('sync', qTn_d.rearrange("h p t -> p h t")[:, :, t0:t0 + 512], qsn[:], reads=[B_qsn],
                      writes=[B_qTn])
                P.dma('sync', qTr_d.rearrange("h p t -> p h t")[:, :, t0:t0 + 512], qsr[:], reads=[B_qsr],
                      writes=[B_qTr])
            P.emit()
            if upto == 'C2b':
                return nc

        with contextlib.ExitStack() as st:
            def sbl(name, shape, dt, st=st):
                return st.enter_context(nc.sbuf_tensor(name, list(shape), dt))
            P = Ph(nc, st)
            qn_ = sbl("d_qn", [128, 16, TOWN], BF16)
            qr_ = sbl("d_qr", [128, 16, TOWN], BF16)
            kpe = sbl("d_kpe", [128, TKV], BF16)
            kT = sbl("d_kT", [128, 2, 2, TKV], BF16)
            vv = sbl("d_vv", [128, 2, 18, 256], BF16)
            pt = sbl("d_pt", [128, 3, 512], BF16)
            rd = sbl("d_rd", [128, 2, 512], F32)
            ao = sbl("d_ao", [128, 2, 512], BF16)
            ps = st.enter_context(nc.psum_tensor("d_ps", [128, 8, 512], F32))
            B_q, B_kpe = Buf(), Buf()
            B_kT = [Buf(), Buf()]
            B_vv = [Buf(), Buf()]
            B_pt = [Buf(), Buf(), Buf()]
            B_rd = [Buf(), Buf()]
            B_ao = [Buf(), Buf()]
            B_ps = [Buf(excl=True) for _ in range(8)]
            P.dma('sync', qn_[:], qTn_d.rearrange("h p t -> p h t"), reads=[B_qTn], writes=[B_q])
            P.memset('vector', qr_[64:128], 0.0, writes=[B_q])
            P.memset('gpsimd', kpe[64:128], 0.0, writes=[B_kpe])
            P.dma('sync', qr_[0:64], qTr_d.rearrange("h p t -> p h t"), reads=[B_qTr], writes=[B_q])
            P.dma('sync', kpe[0:64], kpeT_d, reads=[B_kpeT], writes=[B_kpe])
            v_v = v_d.rearrange("(s p) c -> p s c", p=128)
            it = 0
            pi = 0
            def d_load(hp):
                s = hp % 2
                P.dma('sync', kT[:, s], kT_d[2 * hp:2 * hp + 2].rearrange("h p t -> p h t"), reads=[B_kTd],
                      writes=[B_kT[s]])
                P.dma('sync', vv[:, s], v_v[:, :, hp * 256:(hp + 1) * 256], reads=[B_v], writes=[B_vv[s]])
            d_load(0)
            for hp in range(NH // 2):
                s = hp % 2
                if hp + 1 < NH // 2:
                    d_load(hp + 1)
                for hh in range(2):
                    h = 2 * hp + hh
                    for qb in range(2):
                        q0 = qb * 512
                        o = it % 2
                        bo, bd = 4 + 2 * o, 5 + 2 * o
                        for kt in range(18):
                            bs = pi % 4
                            p3 = pi % 3
                            pi += 1
                            P.mm(ps[:, bs, :], kT[:, s, hh, kt * 128:(kt + 1) * 128], qn_[:, h, q0:q0 + 512], True,
                                 False, reads=[B_kT[s], B_q], writes=[B_ps[bs]])
                            P.mm(ps[:, bs, :], kpe[:, kt * 128:(kt + 1) * 128], qr_[:, h, q0:q0 + 512], False, True,
                                 reads=[B_kpe, B_q], writes=[B_ps[bs]], sig=True)
                            P.act(pt[:, p3, :], ps[:, bs, :], AF.Exp, reads=[B_ps[bs]], writes=[B_pt[p3]])
                            P.mm(ps[:, bo, :], vv[:, s, kt, hh * 128:(hh + 1) * 128], pt[:, p3, :], kt == 0, kt == 17,
                                 reads=[B_vv[s], B_pt[p3]], writes=[B_ps[bo]], sig=(kt == 17))
                            P.mm(ps[:, bd, :], onesb[:], pt[:, p3, :], kt == 0, kt == 17,
                                 reads=[B_const, B_pt[p3]], writes=[B_ps[bd]], sig=True)
                        P.recip(rd[:, o, :], ps[:, bd, :], reads=[B_ps[bd]], writes=[B_rd[o]])
                        P.tt('vector', ao[:, o, :], ps[:, bo, :], rd[:, o, :], ALU.mult,
                             reads=[B_ps[bo], B_rd[o]], writes=[B_ao[o]])
                        P.dma('sync', attnT_d[h * 128:(h + 1) * 128, q0:q0 + 512], ao[:, o, :], reads=[B_ao[o]],
                              writes=[B_attnT])
                        it += 1
            P.emit()
            if upto == 'D':
                return nc

        with contextlib.ExitStack() as st:
            def sbl(name, shape, dt, st=st):
                return st.enter_context(nc.sbuf_tensor(name, list(shape), dt))
            P = Ph(nc, st)
            hTo = sbl("e_hTo", [128, 32, 1056], BF16)
            wcv = sbl("e_wcv", [128, 2, 2, 32, 128], BF16)
            wdw = sbl("e_wdw", [128, 16, 31], F32)
            c3 = sbl("e_c3", [128, 3, 16], F32)
            hm = sbl("e_hm", [128, 32], F32)
            sg = sbl("e_sg", [128, 2, 512], F32)
            hz = sbl("e_hz", [128, 32], F32)
            zc = sbl("e_zc", [128, 2, 1056], BF16)
            dg = sbl("e_dg", [128, 2, 31, 128], BF16)
            cvb = sbl("e_cvb", [128, 16, TOWN], BF16)
            sqb = sbl("e_sqb", [128, 2, 512], BF16)
            st4 = sbl("e_st4", [128, 4, TOWN], F32)
            tmp = sbl("e_tmp", [128, 2, 512], F32)
            cao = sbl("e_cao", [128, 2, TOWN], BF16)
            ps = st.enter_context(nc.psum_tensor("e_ps", [128, 8, 512], F32))
            B_hTo, B_w, B_hm, B_hz, B_cvb, B_st4 = (Buf() for _ in range(6))
            B_wcv = [Buf(), Buf()]
            B_sg = [Buf(), Buf()]
            B_zc = [Buf(), Buf()]
            B_dg = [Buf(), Buf()]
            B_sqb = [Buf(), Buf()]
            B_tmp = [Buf(), Buf()]
            B_cao = [Buf(), Buf()]
            B_ps = [Buf(excl=True) for _ in range(8)]
            P.dma('sync', hTo[:, :, 0:512], hT_v[:, :, 0:512], reads=[B_hT], writes=[B_hTo])
            P.dma('sync', hTo[:, :, 512:1024], hT_v[:, :, 512:1024], reads=[B_hT], writes=[B_hTo])
            P.dma('sync', hTo[:, :, 1024:1056], hT_v[:, :, TKV:TKV + 32], reads=[B_hT], writes=[B_hTo])
            P.dma('sync', wdw[:], wdwT_d, writes=[B_w])
            P.dma('sync', c3[:], cvec3_d, writes=[B_w])
            P.dma('sync', hm[:], hmask_d.broadcast_to([128, 32]), writes=[B_hm])
            segs = [(0, 512, 15), (512, 512, 527), (1024, 32, None)]
            def c3_load(j):
                s = j % 2
                P.dma('gpsimd', wcv[:, s, 0], w_in_v[:, :, CV0 + j * 128:CV0 + (j + 1) * 128], writes=[B_wcv[s]])
                P.dma('gpsimd', wcv[:, s, 1], w_in_v[:, :, CV0 + 2048 + j * 128:CV0 + 2048 + (j + 1) * 128],
                      writes=[B_wcv[s]])
                for k in range(31):
                    P.ts('gpsimd', dg[:, s, k, :], identb[:], wdw[:, j, k:k + 1], ALU.mult, reads=[B_w, B_const],
                         writes=[B_dg[s]])
            c3_load(0)
            for j in range(16):
                s = j % 2
                if j + 1 < 16:
                    c3_load(j + 1)
                for si, (h0, n, zoff) in enumerate(segs):
                    sgi = si % 2
                    for k in range(32):
                        P.mm(ps[:, 0, 0:n], wcv[:, s, 0, k, :], hTo[:, k, h0:h0 + n], k == 0, k == 31,
                             reads=[B_wcv[s], B_hTo], writes=[B_ps[0]], sig=(k == 31))
                    for k in range(32):
                        P.mm(ps[:, 1, 0:n], wcv[:, s, 1, k, :], hTo[:, k, h0:h0 + n], k == 0, k == 31,
                             reads=[B_wcv[s], B_hTo], writes=[B_ps[1]], sig=(k == 31))
                    P.act(sg[:, sgi, 0:n], ps[:, 1, 0:n], AF.Sigmoid, reads=[B_ps[1]], writes=[B_sg[sgi]])
                    if zoff is not None:
                        P.tt('vector', zc[:, s, zoff:zoff + n], ps[:, 0, 0:n], sg[:, sgi, 0:n], ALU.mult,
                             reads=[B_ps[0], B_sg[sgi]], writes=[B_zc[s]])
                    else:
                        P.tt('vector', hz[:, 0:n], ps[:, 0, 0:n], sg[:, sgi, 0:n], ALU.mult,
                             reads=[B_ps[0], B_sg[sgi]], writes=[B_hz])
                        P.tt('vector', zc[:, s, 0:15], hz[:, 0:15], hm[:, 0:15], ALU.mult, reads=[B_hz, B_hm],
                             writes=[B_zc[s]])
                        P.tt('vector', zc[:, s, 1039:1054], hz[:, 15:30], hm[:, 15:30], ALU.mult,
                             reads=[B_hz, B_hm], writes=[B_zc[s]])
                for blk in range(2):
                    bc = 2 + blk
                    for k in range(31):
                        P.mm(ps[:, bc, :], dg[:, s, k, :], zc[:, s, blk * 512 + k:blk * 512 + k + 512], k == 0,
                             k == 30, reads=[B_dg[s], B_zc[s]], writes=[B_ps[bc]], sig=(k == 30))
                    P.act(cvb[:, j, blk * 512:(blk + 1) * 512], ps[:, bc, :], AF.Identity, reads=[B_ps[bc]],
                          writes=[B_cvb], bias=c3[:, 0, j:j + 1])
                    P.act(sqb[:, blk, :], ps[:, bc, :], AF.Square, reads=[B_ps[bc]], writes=[B_sqb[blk]],
                          bias=c3[:, 0, j:j + 1])
                    P.mm(ps[:, 4 + blk, :], onesb[:], cvb[:, j, blk * 512:(blk + 1) * 512], j == 0, j == 15,
                         reads=[B_const, B_cvb], writes=[B_ps[4 + blk]], sig=(j == 15))
                    P.mm(ps[:, 6 + blk, :], onesb[:], sqb[:, blk, :], j == 0, j == 15,
                         reads=[B_const, B_sqb[blk]], writes=[B_ps[6 + blk]], sig=True)
            for blk in range(2):
                c0 = blk * 512
                P.act(st4[:, 0, c0:c0 + 512], ps[:, 4 + blk, :], AF.Identity, reads=[B_ps[4 + blk]], writes=[B_st4],
                      scale=1.0 / 2048)
                P.act(st4[:, 1, c0:c0 + 512], ps[:, 6 + blk, :], AF.Identity, reads=[B_ps[6 + blk]], writes=[B_st4],
                      scale=1.0 / 2048)
                P.tt('vector', st4[:, 2, c0:c0 + 512], st4[:, 0, c0:c0 + 512], st4[:, 0, c0:c0 + 512], ALU.mult,
                     reads=[B_st4], writes=[B_st4])
                P.tt('vector', st4[:, 1, c0:c0 + 512], st4[:, 1, c0:c0 + 512], st4[:, 2, c0:c0 + 512], ALU.subtract,
                     reads=[B_st4], writes=[B_st4])
                P.act(st4[:, 2, c0:c0 + 512], st4[:, 1, c0:c0 + 512], AF.Sqrt, reads=[B_st4], writes=[B_st4],
                      bias=epst[:], scale=1.0)
                P.recip(st4[:, 3, c0:c0 + 512], st4[:, 2, c0:c0 + 512], reads=[B_st4], writes=[B_st4])
            it = 0
            for j in range(16):
                s = j % 2
                for blk in range(2):
                    c0 = blk * 512
                    ti = it % 2
                    it += 1
                    P.tt('vector', tmp[:, ti, :], cvb[:, j, c0:c0 + 512], st4[:, 0, c0:c0 + 512], ALU.subtract,
                         reads=[B_cvb, B_st4], writes=[B_tmp[ti]])
                    P.tt('gpsimd', tmp[:, ti, :], tmp[:, ti, :], st4[:, 3, c0:c0 + 512], ALU.mult,
                         reads=[B_st4], writes=[B_tmp[ti]])
                    P.act(cao[:, s, c0:c0 + 512], tmp[:, ti, :], AF.Silu, reads=[B_tmp[ti], B_w], writes=[B_cao[s]],
                          bias=c3[:, 2, j:j + 1], scale=c3[:, 1, j:j + 1])
                P.dma('sync', caT_d[j * 128:(j + 1) * 128, :], cao[:, s, :], reads=[B_cao[s]], writes=[B_caT])
            P.emit()
            if upto == 'C3':
                return nc

        with contextlib.ExitStack() as st:
            def sbl(name, shape, dt, st=st):
                return st.enter_context(nc.sbuf_tensor(name, list(shape), dt))
            P = Ph(nc, st)
            hTb = sbl("f_hTb", [128, 32, 512], BF16)
            aT = sbl("f_aT", [128, 16, 512], BF16)
            cT = sbl("f_cT", [128, 16, 512], BF16)
            wa = sbl("f_wa", [128, 2, 16, 256], BF16)
            wp = sbl("f_wp", [128, 2, 16, 256], BF16)
            wg = sbl("f_wg", [128, 2, 2, 32, 256], BF16)
            bpw = sbl("f_bpw", [128, 32], F32)
            sgt = sbl("f_sgt", [128, 2, 2, 512], F32)
            mm_ = sbl("f_mm", [128, 2, 2, 512], F32)
            mo = sbl("f_mo", [128, 2, 512], BF16)
            ps = st.enter_context(nc.psum_tensor("f_ps", [128, 8, 512], F32))
            B_in, B_bpw = Buf(), Buf()
            B_wt = [Buf(), Buf()]
            B_sgt = [Buf(), Buf()]
            B_mm = [Buf(), Buf()]
            B_mo = [Buf(), Buf()]
            B_ps = [Buf(excl=True) for _ in range(8)]
            P.dma('sync', bpw[:], bpw_d, writes=[B_bpw])
            woa_v = w_oa.rearrange("(k p) n -> p k n", p=128)
            wpw_v = w_pw.rearrange("(k p) n -> p k n", p=128)
            aT_v = attnT_d.rearrange("(k p) t -> p k t", p=128)
            cT_v = caT_d.rearrange("(k p) t -> p k t", p=128)
            it = 0
            c4_iters = [(blk, jg) for blk in range(2) for jg in range(16)]

            def c4_load(i):
                blk, jg = c4_iters[i]
                ws = i % 2
                g0 = jg * 256
                P.dma('gpsimd', wa[:, ws], woa_v[:, :, g0:g0 + 256], writes=[B_wt[ws]])
                P.dma('gpsimd', wp[:, ws], wpw_v[:, :, g0:g0 + 256], writes=[B_wt[ws]])
                P.dma('gpsimd', wg[:, ws, 0], w_in_v[:, :, G0 + g0:G0 + g0 + 256], writes=[B_wt[ws]])
                P.dma('gpsimd', wg[:, ws, 1], w_in_v[:, :, G0 + D + g0:G0 + D + g0 + 256], writes=[B_wt[ws]])
            c4_load(0)
            for i_, (blk, jg) in enumerate(c4_iters):
                t0 = blk * 512
                ws = i_ % 2
                if jg == 0:
                    P.dma('sync', hTb[:], hT_v[:, :, t0:t0 + 512], reads=[B_hT], writes=[B_in])
                    P.dma('sync', aT[:], aT_v[:, :, t0:t0 + 512], reads=[B_attnT], writes=[B_in])
                    P.dma('sync', cT[:], cT_v[:, :, t0:t0 + 512], reads=[B_caT], writes=[B_in])
                if i_ + 1 < len(c4_iters):
                    c4_load(i_ + 1)
                for jj in range(2):
                    j = jg * 2 + jj
                    s = it % 2
                    it += 1
                    c0 = j * 128
                    cs_ = slice(jj * 128, (jj + 1) * 128)
                    b0 = 4 * s
                    for k in range(16):
                        P.mm(ps[:, b0, :], wa[:, ws, k, cs_], aT[:, k, :], k == 0, k == 15, reads=[B_wt[ws], B_in],
                             writes=[B_ps[b0]], sig=(k == 15))
                    for k in range(16):
                        P.mm(ps[:, b0 + 1, :], wp[:, ws, k, cs_], cT[:, k, :], k == 0, k == 15,
                             reads=[B_wt[ws], B_in], writes=[B_ps[b0 + 1]], sig=(k == 15))
                    for gi in range(2):
                        for k in range(32):
                            P.mm(ps[:, b0 + 2 + gi, :], wg[:, ws, gi, k, cs_], hTb[:, k, :], k == 0, k == 31,
                                 reads=[B_wt[ws], B_in], writes=[B_ps[b0 + 2 + gi]], sig=(k == 31))
                    P.act(sgt[:, s, 0, :], ps[:, b0 + 2, :], AF.Sigmoid, reads=[B_ps[b0 + 2]], writes=[B_sgt[s]])
                    P.act(sgt[:, s, 1, :], ps[:, b0 + 3, :], AF.Sigmoid, reads=[B_ps[b0 + 3]], writes=[B_sgt[s]])
                    P.tt('vector', mm_[:, s, 0, :], ps[:, b0, :], sgt[:, s, 0, :], ALU.mult,
                         reads=[B_ps[b0], B_sgt[s]], writes=[B_mm[s]])
                    P.stt(mm_[:, s, 1, :], ps[:, b0 + 1, :], bpw[:, j:j + 1], sgt[:, s, 1, :], ALU.add, ALU.mult,
                          reads=[B_ps[b0 + 1], B_sgt[s], B_bpw], writes=[B_mm[s]])
                    P.tt('gpsimd', mo[:, s, :], mm_[:, s, 0, :], mm_[:, s, 1, :], ALU.add, reads=[B_mm[s]],
                         writes=[B_mo[s]])
                    P.dma('sync', mT_d[c0:c0 + 128, t0:t0 + 512], mo[:, s, :], reads=[B_mo[s]], writes=[B_mT])
            P.emit()
            if upto == 'C4':
                return nc

        with contextlib.ExitStack() as st:
            def sbl(name, shape, dt, st=st):
                return st.enter_context(nc.sbuf_tensor(name, list(shape), dt))
            P = Ph(nc, st)
            mTb = sbl("g_mTb", [128, 32, 512], BF16)
            wo = sbl("g_wo", [128, 2, 32, 512], BF16)
            ys = sbl("g_ys", [128, 2, 4, 512], F32)
            ps = st.enter_context(nc.psum_tensor("g_ps", [128, 8, 512], F32))
            B_mTb = Buf()
            B_wo = [Buf(), Buf()]
            B_ys = [Buf(), Buf()]
            B_ps = [Buf(excl=True) for _ in range(8)]
            wo_v = w_out.rearrange("(k p) n -> p k n", p=128)
            mT_v = mT_d.rearrange("(k p) t -> p k t", p=128)
            it = 0
            for pa in range(2):
                t0 = pa * 512
                P.dma('sync', mTb[:], mT_v[:, :, t0:t0 + 512], reads=[B_mT], writes=[B_mTb])
                for c in range(8):
                    s = it % 2
                    it += 1
                    P.dma('gpsimd', wo[:, s], wo_v[:, :, c * 512:(c + 1) * 512], writes=[B_wo[s]])
                    for tt_ in range(4):
                        b = 4 * s + tt_
                        for k in range(32):
                            P.mm(ps[:, b, :], mTb[:, k, tt_ * 128:(tt_ + 1) * 128], wo[:, s, k, :], k == 0, k == 31,
                                 reads=[B_mTb, B_wo[s]], writes=[B_ps[b]], sig=(k == 31))
                        P.cp('scalar' if tt_ % 2 == 0 else 'vector', ys[:, s, tt_, :], ps[:, b, :],
                             reads=[B_ps[b]], writes=[B_ys[s]])
                    P.dma('sync', ymoe_d[t0:t0 + 512, c * 512:(c + 1) * 512].rearrange("(t p) n -> p t n", p=128),
                          ys[:, s], reads=[B_ys[s]], writes=[B_ymoe])
            P.emit()
            if upto == 'C5a':
                return nc

        AX = mybir.AxisListType.X
        with contextlib.ExitStack() as st:
            def sbl(name, shape, dt, st=st):
                return st.enter_context(nc.sbuf_tensor(name, list(shape), dt))
            P = Ph(nc, st)
            G1 = sbl("h_G1", [128, D], F32)
            A2 = sbl("h_A2", [128, D], F32)
            B2 = sbl("h_B2", [128, D], F32)
            yb = sbl("h_yb", [128, 2, D], F32)
            xb = sbl("h_xb", [128, 2, D], F32)
            hb = sbl("h_hb", [128, D], BF16)
            junk = sbl("h_junk", [128, D], BF16)
            stat = sbl("h_stat", [128, 6, 8], F32)
            h2Ts = sbl("h_h2Ts", [128, 32, 512], BF16)
            wr = sbl("h_wr", [128, 32, NE], BF16)
            brb = sbl("h_brb", [128, NE], F32)
            rt = sbl("h_rt", [128, 8, NE], F32)
            cmp_ = sbl("h_cmp", [128, 128], F32)
            m8 = sbl("h_m8", [128, 8], F32)
            sm1 = sbl("h_sm1", [128, 4], F32)
            cmT = sbl("h_cmT", [32, TOWN], F32)
            cmTb = sbl("h_cmTb", [32, TOWN], BF16)
            pT = st.enter_context(nc.psum_tensor("h_pT", [128, 4, 8, 128], BF16))
            ps = st.enter_context(nc.psum_tensor("h_ps", [128, 4, 512], F32))
            B_mod, B_hb, B_junk, B_stat, B_h2Ts, B_wr, B_rt, B_m8, B_sm1, B_cmT = (Buf() for _ in range(10))
            B_yb = [Buf(), Buf()]
            B_xb = [Buf(), Buf()]
            B_pT = [Buf(excl=True) for _ in range(4)]
            B_ps = [Buf(excl=True) for _ in range(4)]
            P.dma('sync', G1[:], modv_d[2, 0:1, :].broadcast_to([128, D]), reads=[B_modv], writes=[B_mod])
            P.dma('sync', A2[:], modv_d[4, 0:1, :].broadcast_to([128, D]), reads=[B_modv], writes=[B_mod])
            P.dma('sync', B2[:], modv_d[3, 0:1, :].broadcast_to([128, D]), reads=[B_modv], writes=[B_mod])
            P.dma('gpsimd', wr[:], w_router.rearrange("(k p) n -> p k n", p=128), writes=[B_wr])
            P.dma('sync', brb[:], b_router.broadcast_to([128, NE]), writes=[B_wr])
            P.memset('vector', cmp_[:], 0.0, writes=[B_rt])
            for i in range(8):
                s = i % 2
                r0 = i * 128
                P.dma('sync', yb[:, s, :], ymoe_d[r0:r0 + 128, :], reads=[B_ymoe], writes=[B_yb[s]])
                P.dma('sync', xb[:, s, :], xt_d[r0:r0 + 128, :], writes=[B_xb[s]])
                P.act(junk[:], yb[:, s, :], AF.Square, reads=[B_yb[s]], writes=[B_junk, B_stat],
                      accum_out=stat[:, 0, i:i + 1])
                P.act(stat[:, 1, i:i + 1], stat[:, 0, i:i + 1], AF.Sqrt, reads=[B_stat], writes=[B_stat],
                      bias=epst[:], scale=1.0 / D)
                P.recip(stat[:, 2, i:i + 1], stat[:, 1, i:i + 1], reads=[B_stat], writes=[B_stat])
                P.stt(yb[:, s, :], yb[:, s, :], stat[:, 2, i:i + 1], G1[:], ALU.mult, ALU.mult,
                      reads=[B_stat, B_mod], writes=[B_yb[s]])
                P.tt('gpsimd', xb[:, s, :], xb[:, s, :], yb[:, s, :], ALU.add, reads=[B_yb[s]], writes=[B_xb[s]])
                P.dma('sync', x1_d[r0:r0 + 128, :], xb[:, s, :], reads=[B_xb[s]], writes=[B_x1])
                P.act(junk[:], xb[:, s, :], AF.Square, reads=[B_xb[s]], writes=[B_junk, B_stat],
                      accum_out=stat[:, 3, i:i + 1])
                P.act(stat[:, 4, i:i + 1], stat[:, 3, i:i + 1], AF.Sqrt, reads=[B_stat], writes=[B_stat],
                      bias=epst[:], scale=1.0 / D)
                P.recip(stat[:, 5, i:i + 1], stat[:, 4, i:i + 1], reads=[B_stat], writes=[B_stat])
                P.stt(yb[:, s, :], xb[:, s, :], stat[:, 5, i:i + 1], A2[:], ALU.mult, ALU.mult,
                      reads=[B_stat, B_mod, B_xb[s]], writes=[B_yb[s]])
                P.tt('gpsimd', hb[:], yb[:, s, :], B2[:], ALU.add, reads=[B_yb[s], B_mod], writes=[B_hb])
                tl = i % 4
                for q4 in range(4):
                    for j8 in range(8):
                        j = q4 * 8 + j8
                        P.tr(pT[:, q4, j8, :], hb[:, j * 128:(j + 1) * 128], identb[:], reads=[B_hb],
                             writes=[B_pT[q4]], sig=(j8 == 7))
                    P.cp('scalar' if q4 % 2 == 0 else 'vector', h2Ts[:, q4 * 8:(q4 + 1) * 8, tl * 128:(tl + 1) * 128],
                         pT[:, q4, :, :], reads=[B_pT[q4]], writes=[B_h2Ts])
                pb = i % 2
                for k in range(32):
                    P.mm(ps[:, pb, 0:NE], h2Ts[:, k, tl * 128:(tl + 1) * 128], wr[:, k, :], k == 0, k == 31,
                         reads=[B_h2Ts, B_wr], writes=[B_ps[pb]], sig=(k == 31))
                lg, ex, em, cm = rt[:, 0, :], rt[:, 1, :], rt[:, 2, :], cmp_[:, 0:NE]
                msk = rt[:, 4, :]
                P.tt('vector', lg, ps[:, pb, 0:NE], brb[:], ALU.add, reads=[B_ps[pb], B_wr], writes=[B_rt])
                P.op('vector', lambda e, lg=lg: e.max(out=m8[:], in_=lg), reads=[B_rt], writes=[B_m8])
                P.ts('vector', msk, lg, m8[:, 3:4], ALU.is_ge, reads=[B_m8], writes=[B_rt])
                P.ts('vector', sm1[:, 0:1], m8[:, 0:1], -1.0, ALU.mult, reads=[B_m8], writes=[B_sm1])
                P.act(ex, lg, AF.Exp, reads=[B_rt, B_sm1], writes=[B_rt], bias=sm1[:, 0:1])
                P.tt('vector', em, ex, msk, ALU.mult, reads=[B_rt], writes=[B_rt])
                P.op('vector', lambda e, em=em: e.reduce_sum(out=sm1[:, 1:2], in_=em, axis=AX), reads=[B_rt],
                     writes=[B_sm1])
                P.recip(sm1[:, 2:3], sm1[:, 1:2], reads=[B_sm1], writes=[B_sm1])
                P.ts('vector', cm, em, sm1[:, 2:3], ALU.mult, reads=[B_sm1, B_rt], writes=[B_rt])
                P.tr(ps[:, 2, 0:128], cmp_[:], identf[:], reads=[B_rt, B_const], writes=[B_ps[2]], sig=True)
                P.cp('vector', cmT[:, r0:r0 + 128], ps[0:NE, 2, 0:128], reads=[B_ps[2]], writes=[B_cmT])
                P.cp('scalar', cmTb[:, r0:r0 + 128], ps[0:NE, 2, 0:128], reads=[B_ps[2]], writes=[B_cmT])
                if tl == 3:
                    t0 = (i // 4) * 512
                    P.dma('sync', h2T_d.rearrange("(k p) t -> p k t", p=128)[:, :, t0:t0 + 512], h2Ts[:],
                          reads=[B_h2Ts], writes=[B_h2T])
            P.dma('sync', comb_d, cmT[:], reads=[B_cmT], writes=[B_comb])
            P.dma('sync', combb_d, cmTb[:], reads=[B_cmT], writes=[B_combb])
            P.emit()
            if upto == 'C5b':
                return nc

        with contextlib.ExitStack() as st:
            def sbl(name, shape, dt, st=st):
                return st.enter_context(nc.sbuf_tensor(name, list(shape), dt))
            P = Ph(nc, st)
            h2T = sbl("m_h2T", [128, 32, TOWN], BF16)
            wgu = sbl("m_wgu", [128, 2, 2, 32, 256], BF16)
            bgu = sbl("m_bgu", [128, NE, 24], F32)
            ceb = sbl("m_ceb", [128, 2, TOWN], F32)
            tmp = sbl("m_tmp", [128, 2, 3, 512], F32)
            c7 = sbl("m_c7", [128, 2, 512], F32)
            ao = sbl("m_ao", [128, 2, TOWN], BF16)
            ps = st.enter_context(nc.psum_tensor("m_ps", [128, 8, 512], F32))
            B_h2, B_bgu = Buf(), Buf()
            B_wgu = [Buf(), Buf()]
            B_ceb = [Buf(), Buf()]
            B_tmp = [Buf(), Buf()]
            B_ao = [Buf(), Buf()]
            B_ps = [Buf(excl=True) for _ in range(8)]
            P.dma('sync', h2T[:, :, 0:512], h2T_d.rearrange("(k p) t -> p k t", p=128)[:, :, 0:512], reads=[B_h2T],
                  writes=[B_h2])
            P.dma('sync', h2T[:, :, 512:1024], h2T_d.rearrange("(k p) t -> p k t", p=128)[:, :, 512:1024],
                  reads=[B_h2T], writes=[B_h2])
            P.dma('sync', bgu[:], bgu_d, writes=[B_bgu])
            P.memset('vector', c7[:, 0, :], 7.0, writes=[B_bgu])
            P.memset('vector', c7[:, 1, :], 1.0, writes=[B_bgu])
            ti = 0
            fi_ = 0
            iters = [(e, fb) for e in range(KNE) for fb in range(6)]

            def e1_load(i):
                e, fb = iters[i]
                ws = i % 2
                if fb == 0:
                    P.dma('sync', ceb[:, e % 2, :], comb_d[e:e + 1, :].broadcast_to([128, TOWN]), reads=[B_comb],
                          writes=[B_ceb[e % 2]])
                wgu_v = w_gu[e].rearrange("(k p) n -> p k n", p=128)
                P.dma('gpsimd', wgu[:, ws, 0], wgu_v[:, :, fb * 256:(fb + 1) * 256], writes=[B_wgu[ws]])
                P.dma('gpsimd', wgu[:, ws, 1], wgu_v[:, :, FF + fb * 256:FF + (fb + 1) * 256], writes=[B_wgu[ws]])
            e1_load(0)
            for i_, (e, fb) in enumerate(iters):
                if i_ + 1 < len(iters):
                    e1_load(i_ + 1)
                ce = e % 2
                ws = i_ % 2
                if True:
                    for fi in range(2):
                        f = fb * 2 + fi
                        asl = fi_ % 2
                        fi_ += 1
                        for half in range(2):
                            h0 = half * 512
                            pp = ti % 4
                            tb = ti % 2
                            ti += 1
                            bg_, bu_ = 2 * pp, 2 * pp + 1
                            for k in range(32):
                                P.mm(ps[:, bg_, :], wgu[:, ws, 0, k, fi * 128:(fi + 1) * 128], h2T[:, k, h0:h0 + 512],
                                     k == 0, k == 31, reads=[B_wgu[ws], B_h2], writes=[B_ps[bg_]], sig=(k == 31))
                            for k in range(32):
                                P.mm(ps[:, bu_, :], wgu[:, ws, 1, k, fi * 128:(fi + 1) * 128], h2T[:, k, h0:h0 + 512],
                                     k == 0, k == 31, reads=[B_wgu[ws], B_h2], writes=[B_ps[bu_]], sig=(k == 31))
                            gc, sg_, uc = tmp[:, tb, 0, :], tmp[:, tb, 1, :], tmp[:, tb, 2, :]
                            P.stt(gc, ps[:, bg_, :], bgu[:, e, f:f + 1], c7[:, 0, :], ALU.add, ALU.min,
                                  reads=[B_ps[bg_], B_bgu], writes=[B_tmp[tb]])
                            P.act(sg_, gc, AF.Sigmoid, reads=[B_tmp[tb]], writes=[B_tmp[tb]], scale=1.702)
                            P.stt(uc, ps[:, bu_, :], bgu[:, e, 12 + f:13 + f], c7[:, 0, :], ALU.add, ALU.min,
                                  reads=[B_ps[bu_], B_bgu], writes=[B_tmp[tb]])
                            P.stt(uc, uc, -7.0, c7[:, 1, :], ALU.max, ALU.add, reads=[B_tmp[tb], B_bgu],
                                  writes=[B_tmp[tb]])
                            P.tt('gpsimd', gc, gc, sg_, ALU.mult, reads=[B_tmp[tb]], writes=[B_tmp[tb]])
                            P.tt('gpsimd', gc, gc, uc, ALU.mult, reads=[B_tmp[tb]], writes=[B_tmp[tb]])
                            P.tt('vector', ao[:, asl, h0:h0 + 512], gc, ceb[:, ce, h0:h0 + 512], ALU.mult,
                                 reads=[B_tmp[tb], B_ceb[ce]], writes=[B_ao[asl]])
                        r0 = e * FF + f * 128
                        P.dma('sync', actT_d[r0:r0 + 128, :], ao[:, asl, :], reads=[B_ao[asl]], writes=[B_actT])
            P.emit()
            if upto == 'E1':
                return nc

        with contextlib.ExitStack() as st:
            def sbl(name, shape, dt, st=st):
                return st.enter_context(nc.sbuf_tensor(name, list(shape), dt))
            P = Ph(nc, st)
            aTb = sbl("n_aT", [128, 3, 12, TOWN], BF16)
            wd = sbl("n_wd", [128, 3, 12, 512], BF16)
            cb = sbl("n_cb", [128, TOWN], BF16)
            bd = sbl("n_bd", [128, D], BF16)
            ys = sbl("n_ys", [128, 8, 512], F32)
            ps = st.enter_context(nc.psum_tensor("n_ps", [128, 8, 512], F32))
            B_cb, B_ys = Buf(), Buf()
            B_aT = [Buf(), Buf(), Buf()]
            B_wd = [Buf(), Buf(), Buf()]
            B_ps = [Buf(excl=True) for _ in range(8)]
            P.memset('vector', cb[:], 0.0, writes=[B_cb])
            P.memset('gpsimd', bd[:], 0.0, writes=[B_cb])
            P.dma('sync', cb[0:32], combb_d, reads=[B_combb], writes=[B_cb])
            P.dma('gpsimd', bd[0:32, 0:2048], b_down[:, 0:2048], writes=[B_cb])
            P.dma('gpsimd', bd[0:32, 2048:4096], b_down[:, 2048:4096], writes=[B_cb])
            it = 0
            for c in range(8):
                for e in range(KNE):
                    s = it % 3
                    it += 1
                    P.dma('sync', aTb[:, s], actT_d[e * FF:(e + 1) * FF, :].rearrange("(g p) t -> p g t", p=128),
                          reads=[B_actT], writes=[B_aT[s]])
                    P.dma('gpsimd', wd[:, s],
                          w_down[e * FF:(e + 1) * FF, c * 512:(c + 1) * 512].rearrange("(g p) n -> p g n", p=128),
                          writes=[B_wd[s]])
                    for g in range(12):
                        for tt_ in range(8):
                            P.mm(ps[:, tt_, :], aTb[:, s, g, tt_ * 128:(tt_ + 1) * 128], wd[:, s, g, :],
                                 e == 0 and g == 0, False, reads=[B_aT[s], B_wd[s]], writes=[B_ps[tt_]],
                                 sig=(g == 11 and tt_ == 7))
                for tt_ in range(8):
                    P.mm(ps[:, tt_, :], cb[:, tt_ * 128:(tt_ + 1) * 128], bd[:, c * 512:(c + 1) * 512], False, True,
                         reads=[B_cb], writes=[B_ps[tt_]], sig=True)
                    P.cp('scalar' if tt_ % 2 == 0 else 'vector', ys[:, tt_, :], ps[:, tt_, :], reads=[B_ps[tt_]],
                         writes=[B_ys])
                P.dma('sync', ymoe_d[:, c * 512:(c + 1) * 512].rearrange("(t p) n -> p t n", p=128), ys[:],
                      reads=[B_ys], writes=[B_ymoe])
            P.emit()
            if upto == 'E2':
                return nc

        with contextlib.ExitStack() as st:
            def sbl(name, shape, dt, st=st):
                return st.enter_context(nc.sbuf_tensor(name, list(shape), dt))
            P = Ph(nc, st)
            G2 = sbl("o_G2", [128, D], F32)
            yb = sbl("o_yb", [128, 2, D], F32)
            xb = sbl("o_xb", [128, 2, D], F32)
            junk = sbl("o_junk", [128, D], BF16)
            stat = sbl("o_stat", [128, 3, 8], F32)
            B_mod, B_junk, B_stat = Buf(), Buf(), Buf()
            B_yb = [Buf(), Buf()]
            B_xb = [Buf(), Buf()]
            P.dma('sync', G2[:], modv_d[5, 0:1, :].broadcast_to([128, D]), reads=[B_modv], writes=[B_mod])
            for i in range(8):
                s = i % 2
                r0 = i * 128
                P.dma('sync', yb[:, s, :], ymoe_d[r0:r0 + 128, :], reads=[B_ymoe], writes=[B_yb[s]])
                P.dma('sync', xb[:, s, :], x1_d[r0:r0 + 128, :], reads=[B_x1], writes=[B_xb[s]])
                P.act(junk[:], yb[:, s, :], AF.Square, reads=[B_yb[s]], writes=[B_junk, B_stat],
                      accum_out=stat[:, 0, i:i + 1])
                P.act(stat[:, 1, i:i + 1], stat[:, 0, i:i + 1], AF.Sqrt, reads=[B_stat], writes=[B_stat],
                      bias=epst[:], scale=1.0 / D)
                P.recip(stat[:, 2, i:i + 1], stat[:, 1, i:i + 1], reads=[B_stat], writes=[B_stat])
                P.stt(yb[:, s, :], yb[:, s, :], stat[:, 2, i:i + 1], G2[:], ALU.mult, ALU.mult,
                      reads=[B_stat, B_mod], writes=[B_yb[s]])
                P.tt('gpsimd', xb[:, s, :], xb[:, s, :], yb[:, s, :], ALU.add, reads=[B_yb[s]], writes=[B_xb[s]])
                P.dma('sync', out_d[r0:r0 + 128, :], xb[:, s, :], reads=[B_xb[s]], writes=[B_out])
            P.emit()
            if upto == 'F':
                return nc
    return nc


def _rope_tables(pos):
    rows = (pos // 64).astype(np.float32)
    cols = (pos % 64).astype(np.float32)
    n_pairs = 16
    inv = (np.float32(10000.0) ** (-np.arange(n_pairs, dtype=np.float32) / np.float32(n_pairs))).astype(np.float32)
    ang = np.concatenate([rows[:, None] * inv[None, :], cols[:, None] * inv[None, :]], axis=-1).astype(np.float32)
    return np.cos(ang).astype(np.float32), np.sin(ang).astype(np.float32)


def _core_inputs(core, x, c, ctx, c_ctx, shared):
    b, hf = core // 2, core % 2
    o0 = hf * TOWN
    p0 = (1 - hf) * TOWN
    halo = np.zeros((32, D), np.float32)
    hmask = np.zeros((1, 32), np.float32)
    if hf == 1:
        halo[0:15] = x[b, o0 - 15:o0]
        hmask[0, 0:15] = 1.0
    else:
        halo[15:30] = x[b, o0 + TOWN:o0 + TOWN + 15]
        hmask[0, 15:30] = 1.0
    xt = np.concatenate([x[b, o0:o0 + TOWN], x[b, p0:p0 + TOWN], ctx[b], halo], axis=0)
    pos = np.concatenate([np.arange(o0, o0 + TOWN), np.arange(p0, p0 + TOWN)])
    cs, sn = _rope_tables(pos)
    cosT = np.ones((64, TKV), np.float32)
    sinT = np.zeros((64, TKV), np.float32)
    cosT[0:32, 0:2048] = cs.T
    cosT[32:64, 0:2048] = cs.T
    sinT[0:32, 0:2048] = sn.T
    sinT[32:64, 0:2048] = sn.T
    cv = np.stack([c[b], c_ctx], axis=-1)
    cvT = np.ascontiguousarray(cv.reshape(32, 128, 2).transpose(1, 0, 2))
    m = dict(shared)
    m.update(xt=np.ascontiguousarray(xt), cvT=cvT, hmask=hmask, cosT=cosT, sinT=sinT)
    return m


def _pj(v, nchunk):
    return np.ascontiguousarray(np.asarray(v, np.float32).reshape(nchunk, 128).T)


def _shared_inputs(w_ada, b_ada, pre1_g, post1_g, pre2_g, post2_g, w_in, q_norm_g, w_uq, kv_norm_g, w_ukv,
                   w_o_attn, w_dw, b_dw, cln_g, cln_b, w_pw, b_pw, w_out, w_router, b_router, w_gu, b_gu,
                   w_down, b_down):
    f = lambda a: np.ascontiguousarray(np.asarray(a, np.float32))
    return dict(
        ident=np.eye(128, dtype=np.float32),
        w_ada=f(w_ada[0]), b_ada=f(b_ada[0].reshape(6, D)),
        gains=f(np.stack([pre1_g[0], post1_g[0], pre2_g[0], post2_g[0]])),
        w_in=f(w_in[0]), qng=_pj(q_norm_g[0], 8), w_uq=f(w_uq[0]), kvg=_pj(kv_norm_g[0], 4), w_ukv=f(w_ukv[0]),
        w_o_attn=f(w_o_attn[0]),
        wdwT=f(np.asarray(w_dw[0]).T.reshape(16, 128, 31).transpose(1, 0, 2)),
        cvec3=f(np.stack([_pj(b_dw[0], 16), _pj(cln_g[0], 16), _pj(cln_b[0], 16)], axis=1)),
        w_pw=f(w_pw[0]), bpw=_pj(b_pw[0], 32), w_out=f(w_out[0]), w_router=f(w_router[0]),
        b_router=f(b_router[0].reshape(1, NE)), w_gu=f(w_gu[0]),
        bgu=f(np.asarray(b_gu[0]).reshape(NE, 24, 128).transpose(2, 0, 1)),
        w_down=f(np.asarray(w_down[0]).reshape(-1, D)), b_down=f(b_down[0]),
    )


_NC_CACHE = {}


def kernel(x, c, ctx, c_ctx, w_ada, b_ada, pre1_g, post1_g, pre2_g, post2_g, w_in, q_norm_g, w_uq, kv_norm_g,
           w_ukv, w_o_attn, w_dw, b_dw, cln_g, cln_b, w_pw, b_pw, w_out, w_router, b_router, w_gu, b_gu,
           w_down, b_down):
    x = np.asarray(x, np.float32)
    c = np.asarray(c, np.float32)
    ctx = np.asarray(ctx, np.float32)
    c_ctx = np.asarray(c_ctx, np.float32)
    shared = _shared_inputs(w_ada, b_ada, pre1_g, post1_g, pre2_g, post2_g, w_in, q_norm_g, w_uq, kv_norm_g, w_ukv,
                            w_o_attn, w_dw, b_dw, cln_g, cln_b, w_pw, b_pw, w_out, w_router, b_router, w_gu, b_gu,
                            w_down, b_down)
    if 'nc' not in _NC_CACHE:
        _NC_CACHE['nc'] = build_nc()
    nc = _NC_CACHE['nc']
    in_maps = [_core_inputs(i, x, c, ctx, c_ctx, shared) for i in range(8)]
    res = run_bass_kernel_spmd(nc, in_maps, core_ids=list(range(8)))
    out = np.empty((4, 2048, D), np.float32)
    for i in range(8):
        b, hf = i // 2, i % 2
        out[b, hf * TOWN:(hf + 1) * TOWN] = np.asarray(res.results[i]["out"], np.float32)
    return out
```
